# Optimizing a Trainium2 kernel written in Bass

```python
import math
import jax, jax.numpy as jnp
from jax import lax
import numpy as np

D_MODEL = 4096
BATCH = 4
SEQ = 2048
DEPTH = 2

GRID_W = 64
CTX_LEN = 256
N_EVEN = (DEPTH + 1) // 2
N_ODD = DEPTH // 2
NORM_EPS = 1e-6

POOL_WINDOWS = (2, 4, 8, 16)
POOL_GROUP = D_MODEL // 16
POOL_WIDTH = len(POOL_WINDOWS) * POOL_GROUP
GDN_HEAD_DIM = 128
GDN_HEADS = (D_MODEL - POOL_WIDTH) // GDN_HEAD_DIM
GDN_WIDTH = GDN_HEADS * GDN_HEAD_DIM
GDN_CONV = 5
GDN_CHUNK = 64
QKV_COLS = 3 * GDN_WIDTH
STATE_COLS = QKV_COLS + 4 * GDN_HEADS
IN_COLS = STATE_COLS + POOL_WIDTH + GDN_WIDTH
MIX_WIDTH = POOL_WIDTH + GDN_WIDTH
DIFF_HEAD_DIM = 128
DIFF_HEADS = D_MODEL // (2 * DIFF_HEAD_DIM)
ROPE_BASE = 10000.0
Q_BLOCK = 128
N_EXPERTS = 16
EC_CAPACITY = 2
D_EXPERT = 5 * D_MODEL // 16

kernel_name = 'hybrid_pool_gdn_diffattn_ecmoe_dit'


def rmsnorm(x, g):
    xf = x.astype(jnp.float32)
    y = xf * lax.rsqrt(jnp.mean(xf * xf, axis=-1, keepdims=True) + NORM_EPS)
    return (y * g.astype(jnp.float32)).astype(x.dtype)


def modulate(h, shift, scale):
    return h * (1 + scale) + shift


def l2norm(t):
    tf = t.astype(jnp.float32)
    return (tf * lax.rsqrt(jnp.sum(tf * tf, axis=-1, keepdims=True) + NORM_EPS)).astype(t.dtype)


def centred_mean(u, w):
    B, L, C = u.shape
    cs = jnp.concatenate([jnp.zeros((B, 1, C), jnp.float32), jnp.cumsum(u.astype(jnp.float32), axis=1)], axis=1)
    t = jnp.arange(L)
    lo = jnp.clip(t - w // 2, 0, L)
    hi = jnp.clip(t + w // 2, 0, L)
    cnt = (hi - lo).astype(jnp.float32)
    return ((cs[:, hi] - cs[:, lo]) / cnt[None, :, None]).astype(u.dtype)


def pool_mix(u, pool_w, pool_scale):
    B, L, _ = u.shape
    ug = u.reshape(B, L, len(POOL_WINDOWS), POOL_GROUP)
    means = jnp.stack([centred_mean(ug[:, :, i], w) for i, w in enumerate(POOL_WINDOWS)], axis=2)
    y = jnp.einsum('blgc,gce->blge', means - ug, pool_w).reshape(B, L, POOL_WIDTH)
    return y * pool_scale


def short_conv(u, w):
    y = lax.conv_general_dilated(u, w.astype(u.dtype)[:, None, :], window_strides=(1,),
                                 padding=[(GDN_CONV // 2, GDN_CONV // 2)],
                                 dimension_numbers=('NWC', 'WIO', 'NWC'),
                                 feature_group_count=u.shape[-1])
    return jax.nn.silu(y)


def gated_delta_chunked(q, k, v, g, beta, s0):
    out_dtype = v.dtype
    B, L, H, dk = q.shape
    dv = v.shape[-1]
    C = GDN_CHUNK
    N = L // C
    f32 = jnp.float32

    def chunked(t):
        t = t.astype(f32).reshape(B, N, C, H, *t.shape[3:])
        return jnp.moveaxis(t, 3, 1)

    q = chunked(q) * dk ** -0.5
    k = chunked(k)
    v = chunked(v)
    beta = chunked(beta)
    g = jnp.cumsum(chunked(g), axis=-1)
    pos = jnp.arange(C)
    lower = pos[:, None] >= pos[None, :]
    strict = pos[:, None] > pos[None, :]
    decay = jnp.exp(jnp.where(lower, g[..., :, None] - g[..., None, :], -jnp.inf))
    kb = k * beta[..., None]
    a_mat = jnp.where(strict, jnp.einsum('bhnid,bhnjd->bhnij', kb, k) * decay, 0.0)
    rhs = jnp.concatenate([v * beta[..., None], kb * jnp.exp(g)[..., None]], axis=-1)
    sol = lax.linalg.triangular_solve(a_mat + jnp.eye(C, dtype=f32), rhs, left_side=True, lower=True,
                                      unit_diagonal=True)
    u, w = sol[..., :dv], sol[..., dv:]
    attn = jnp.einsum('bhnid,bhnjd->bhnij', q, k) * decay
    q_dec = q * jnp.exp(g)[..., None]
    k_dec = k * jnp.exp(g[..., -1:] - g)[..., None]
    g_end = jnp.exp(g[..., -1])

    def step(s, inp):
        u_n, w_n, q_n, k_n, a_n, ge_n = inp
        v_new = u_n - jnp.einsum('bhcd,bhde->bhce', w_n, s)
        o_n = jnp.einsum('bhcd,bhde->bhce', q_n, s) + jnp.einsum('bhij,bhje->bhie', a_n, v_new)
        s = s * ge_n[..., None, None] + jnp.einsum('bhcd,bhce->bhde', k_n, v_new)
        return s, o_n

    xs = tuple(jnp.moveaxis(t, 2, 0) for t in (u, w, q_dec, k_dec, attn, g_end))
    s_end, o = lax.scan(step, s0, xs)
    o = jnp.transpose(o, (1, 0, 3, 2, 4)).reshape(B, L, H, dv)
    return o.astype(out_dtype), s_end


def bidir_delta(q, k, v, g_f, beta_f, g_b, beta_b, s_f, s_b):
    o_f, s_f = gated_delta_chunked(q, k, v, g_f, beta_f, s_f)
    flip = lambda t: jnp.flip(t, axis=1)
    o_b, s_b = gated_delta_chunked(flip(q), flip(k), flip(v), flip(g_b), flip(beta_b), s_b)
    return o_f + flip(o_b), s_f, s_b


def pool_gdn_mixer(h_ctx, h_lat, w_in, pool_w, pool_scale, conv_w, a_log_f, a_log_b, dt_bias_f, dt_bias_b,
                   gdn_norm, w_out, need_ctx_out):
    f32 = jnp.float32

    def gdn_inputs(p):
        B, L, _ = p.shape
        qkv = short_conv(p[..., :QKV_COLS], conv_w)
        q, k, v = jnp.split(qkv, 3, axis=-1)
        q = l2norm(q.reshape(B, L, GDN_HEADS, GDN_HEAD_DIM))
        k = l2norm(k.reshape(B, L, GDN_HEADS, GDN_HEAD_DIM))
        v = v.reshape(B, L, GDN_HEADS, GDN_HEAD_DIM)
        a_f, a_b, b_f, b_b = jnp.split(p[..., QKV_COLS:STATE_COLS].astype(f32), 4, axis=-1)
        g_f = -jnp.exp(a_log_f.astype(f32)) * jax.nn.softplus(a_f + dt_bias_f.astype(f32))
        g_b = -jnp.exp(a_log_b.astype(f32)) * jax.nn.softplus(a_b + dt_bias_b.astype(f32))
        return q, k, v, g_f, jax.nn.sigmoid(b_f), g_b, jax.nn.sigmoid(b_b)

    def merge_out(p, o):
        B, L, _ = p.shape
        z = p[..., STATE_COLS + POOL_WIDTH:].reshape(B, L, GDN_HEADS, GDN_HEAD_DIM)
        y_gdn = (rmsnorm(o, gdn_norm) * jax.nn.silu(z)).reshape(B, L, GDN_WIDTH)
        y_pool = pool_mix(p[..., STATE_COLS:STATE_COLS + POOL_WIDTH], pool_w, pool_scale)
        return jnp.concatenate([y_pool, y_gdn], axis=-1) @ w_out

    B = h_lat.shape[0]
    zero = jnp.zeros((B, GDN_HEADS, GDN_HEAD_DIM, GDN_HEAD_DIM), f32)
    p_ctx = h_ctx @ (w_in if need_ctx_out else w_in[:, :STATE_COLS])
    o_ctx, s_f, s_b = bidir_delta(*gdn_inputs(p_ctx), zero, zero)
    p_lat = h_lat @ w_in
    o_lat, _, _ = bidir_delta(*gdn_inputs(p_lat), s_f, s_b)
    y_lat = merge_out(p_lat, o_lat)
    y_ctx = merge_out(p_ctx, o_ctx) if need_ctx_out else None
    return y_ctx, y_lat


def axial_rope_tables(rows):
    pos_row = jnp.repeat(jnp.arange(rows), GRID_W).astype(jnp.float32)
    pos_col = jnp.tile(jnp.arange(GRID_W), rows).astype(jnp.float32)
    quarter = DIFF_HEAD_DIM // 4
    inv_freq = ROPE_BASE ** (-jnp.arange(quarter, dtype=jnp.float32) / quarter)
    ang_r = pos_row[:, None] * inv_freq[None, :]
    ang_c = pos_col[:, None] * inv_freq[None, :]
    ang = jnp.concatenate([ang_r, ang_r, ang_c, ang_c], axis=-1)
    return jnp.cos(ang), jnp.sin(ang)


def axial_rope(x, cos, sin):
    quarter = DIFF_HEAD_DIM // 4
    xs = x.reshape(*x.shape[:-1], 2, 2, quarter)
    rot = jnp.stack([-xs[..., 1, :], xs[..., 0, :]], axis=-2).reshape(x.shape)
    return x * cos[None, :, None, None, :] + rot * sin[None, :, None, None, :]


def diff_attention(q, k, v, lam):
    B, Lq, H, _, d = q.shape
    nb = Lq // Q_BLOCK
    qb = jnp.moveaxis(q.reshape(B, nb, Q_BLOCK, H, 2, d), 1, 0)
    scale = d ** -0.5

    def block(q_blk):
        s = jnp.einsum('bqhmd,bkhmd->bhmqk', q_blk, k).astype(jnp.float32) * scale
        p = jax.nn.softmax(s, axis=-1)
        a = p[:, :, 0] - lam * p[:, :, 1]
        return jnp.einsum('bhqk,bkhe->bqhe', a.astype(v.dtype), v)

    o = lax.map(block, qb)
    return jnp.moveaxis(o, 0, 1).reshape(B, Lq, H, 2 * d)


def diff_attn_mixer(h_ctx, h_lat, w_qkv, lam_q1, lam_k1, lam_q2, lam_k2, subln, w_out, lambda_init, cos, sin,
                    need_ctx_out):
    f32 = jnp.float32
    lam = (jnp.exp(jnp.sum(lam_q1.astype(f32) * lam_k1.astype(f32)))
           - jnp.exp(jnp.sum(lam_q2.astype(f32) * lam_k2.astype(f32))) + lambda_init)
    qk_heads = lambda t: t.reshape(t.shape[0], t.shape[1], DIFF_HEADS, 2, DIFF_HEAD_DIM)
    v_heads = lambda t: t.reshape(t.shape[0], t.shape[1], DIFF_HEADS, 2 * DIFF_HEAD_DIM)

    def finish(o):
        B, L = o.shape[:2]
        o = rmsnorm(o, subln) * (1 - lambda_init)
        return o.reshape(B, L, D_MODEL) @ w_out

    q_l, k_l, v_l = jnp.split(h_lat @ w_qkv, 3, axis=-1)
    q_l = axial_rope(qk_heads(q_l), cos, sin)
    k_l = axial_rope(qk_heads(k_l), cos, sin)
    v_l = v_heads(v_l)
    k_c, v_c = jnp.split(h_ctx @ w_qkv[:, D_MODEL:], 2, axis=-1)
    k_c, v_c = qk_heads(k_c), v_heads(v_c)
    y_lat = finish(diff_attention(q_l, jnp.concatenate([k_l, k_c], axis=1),
                                  jnp.concatenate([v_l, v_c], axis=1), lam))
    y_ctx = finish(diff_attention(qk_heads(h_ctx @ w_qkv[:, :D_MODEL]), k_c, v_c, lam)) if need_ctx_out else None
    return y_ctx, y_lat


def expert_choice_ffn(h, router, w1, w3, w2):
    B, L, _ = h.shape
    cap = EC_CAPACITY * L // N_EXPERTS
    aff = jax.nn.softmax((h @ router).astype(jnp.float32), axis=-1)
    gates, idx = lax.top_k(jnp.transpose(aff, (0, 2, 1)), cap)
    bidx = jnp.arange(B)[:, None, None]
    xe = h[bidx, idx]
    hid = jax.nn.silu(jnp.einsum('becd,edf->becf', xe, w1)) * jnp.einsum('becd,edf->becf', xe, w3)
    ye = jnp.einsum('becf,efd->becd', hid, w2) * gates[..., None].astype(h.dtype)
    return jnp.zeros_like(h).at[bidx, idx].add(ye)


def setup_inputs(seed: int = 0) -> dict:
    key = jax.random.key(seed)
    k = jax.random.split(key, 32)
    f32 = jnp.float32
    D = D_MODEL

    def nrm(i, shape, scale):
        return jax.random.normal(k[i], shape, f32) * scale

    def gain(i, shape):
        return 1.0 + nrm(i, shape, 0.05)

    a_log = jnp.log(jax.random.uniform(k[11], (2, N_EVEN, GDN_HEADS), f32, minval=1.0, maxval=16.0))
    dt = jnp.exp(jax.random.uniform(k[12], (2, N_EVEN, GDN_HEADS), f32,
                                    minval=math.log(1e-3), maxval=math.log(1e-1)))
    dt_bias = dt + jnp.log(-jnp.expm1(-dt))
    return {
        'x': nrm(0, (BATCH, SEQ, D), 1.0),
        'c': nrm(1, (BATCH, D), 1.0),
        'ctx': nrm(2, (BATCH, CTX_LEN, D), 1.0),
        'c_ctx': nrm(3, (D,), 1.0),
        'ada_w': nrm(4, (DEPTH, D, 6 * D), 0.5 * D ** -0.5),
        'ada_b': nrm(5, (DEPTH, 6 * D), 0.02),
        'norm_mix': gain(6, (DEPTH, D)),
        'norm_ffn': gain(7, (DEPTH, D)),
        'w_in': nrm(8, (N_EVEN, D, IN_COLS), D ** -0.5),
        'pool_w': nrm(9, (N_EVEN, len(POOL_WINDOWS), POOL_GROUP, POOL_GROUP), POOL_GROUP ** -0.5),
        'pool_scale': gain(10, (N_EVEN, POOL_WIDTH)),
        'conv_w': nrm(13, (N_EVEN, GDN_CONV, QKV_COLS), GDN_CONV ** -0.5),
        'a_log_f': a_log[0],
        'a_log_b': a_log[1],
        'dt_bias_f': dt_bias[0],
        'dt_bias_b': dt_bias[1],
        'gdn_norm': gain(14, (N_EVEN, GDN_HEAD_DIM)),
        'w_out_ab': nrm(15, (N_EVEN, MIX_WIDTH, D), MIX_WIDTH ** -0.5),
        'w_qkv': nrm(16, (N_ODD, D, 3 * D), D ** -0.5),
        'lam_q1': nrm(17, (N_ODD, DIFF_HEAD_DIM), 0.1),
        'lam_k1': nrm(18, (N_ODD, DIFF_HEAD_DIM), 0.1),
        'lam_q2': nrm(19, (N_ODD, DIFF_HEAD_DIM), 0.1),
        'lam_k2': nrm(20, (N_ODD, DIFF_HEAD_DIM), 0.1),
        'subln': gain(21, (N_ODD, 2 * DIFF_HEAD_DIM)),
        'w_out_c': nrm(22, (N_ODD, D, D), D ** -0.5),
        'router': nrm(23, (DEPTH, D, N_EXPERTS), D ** -0.5),
        'w1': nrm(24, (DEPTH, N_EXPERTS, D, D_EXPERT), D ** -0.5),
        'w3': nrm(25, (DEPTH, N_EXPERTS, D, D_EXPERT), D ** -0.5),
        'w2': nrm(26, (DEPTH, N_EXPERTS, D_EXPERT, D), D_EXPERT ** -0.5),
        'final_norm': gain(27, (D,)),
    }


def reference(x, c, ctx, c_ctx, ada_w, ada_b, norm_mix, norm_ffn,
              w_in, pool_w, pool_scale, conv_w, a_log_f, a_log_b, dt_bias_f, dt_bias_b, gdn_norm, w_out_ab,
              w_qkv, lam_q1, lam_k1, lam_q2, lam_k2, subln, w_out_c,
              router, w1, w3, w2, final_norm):
    rows = x.shape[1] // GRID_W
    cos, sin = axial_rope_tables(rows)
    cos, sin = cos.astype(x.dtype), sin.astype(x.dtype)
    cond_lat = jax.nn.silu(c)
    cond_ctx = jax.nn.silu(c_ctx)[None]
    x_lat, x_ctx = x, ctx
    for layer in range(DEPTH):
        last = layer == DEPTH - 1
        j = layer // 2
        m_lat = jnp.split((cond_lat @ ada_w[layer] + ada_b[layer])[:, None, :], 6, axis=-1)
        m_ctx = jnp.split((cond_ctx @ ada_w[layer] + ada_b[layer])[:, None, :], 6, axis=-1)
        h_lat = modulate(rmsnorm(x_lat, norm_mix[layer]), m_lat[0], m_lat[1])
        h_ctx = modulate(rmsnorm(x_ctx, norm_mix[layer]), m_ctx[0], m_ctx[1])
        if layer % 2 == 0:
            y_ctx, y_lat = pool_gdn_mixer(h_ctx, h_lat, w_in[j], pool_w[j], pool_scale[j], conv_w[j],
                                          a_log_f[j], a_log_b[j], dt_bias_f[j], dt_bias_b[j], gdn_norm[j],
                                          w_out_ab[j], not last)
        else:
            lambda_init = 0.8 - 0.6 * math.exp(-0.3 * layer)
            y_ctx, y_lat = diff_attn_mixer(h_ctx, h_lat, w_qkv[j], lam_q1[j], lam_k1[j], lam_q2[j], lam_k2[j],
                                           subln[j], w_out_c[j], lambda_init, cos, sin, not last)
        x_lat = x_lat + m_lat[2] * y_lat
        h2_lat = modulate(rmsnorm(x_lat, norm_ffn[layer]), m_lat[3], m_lat[4])
        x_lat = x_lat + m_lat[5] * expert_choice_ffn(h2_lat, router[layer], w1[layer], w3[layer], w2[layer])
        if not last:
            x_ctx = x_ctx + m_ctx[2] * y_ctx
            h2_ctx = modulate(rmsnorm(x_ctx, norm_ffn[layer]), m_ctx[3], m_ctx[4])
            x_ctx = x_ctx + m_ctx[5] * expert_choice_ffn(h2_ctx, router[layer], w1[layer], w3[layer], w2[layer])
    return rmsnorm(x_lat, final_norm)
```

```python
import math
import os
import numpy as np
from contextlib import ExitStack, contextmanager
import concourse.bass as bass
import concourse.mybir as mybir
from concourse.bass_utils import run_bass_kernel_spmd

F32 = mybir.dt.float32
BF16 = mybir.dt.bfloat16
AF = mybir.ActivationFunctionType
ALU = mybir.AluOpType
AX = mybir.AxisListType
NCORES = 8
N_DMA_SEMS = 10
EPS = 1e-6


class Buf:
    __slots__ = ("t", "name", "lastw", "readers", "isdram", "base", "ispsum")

    def __init__(self, t, name, isdram=False, base=None, ispsum=False):
        self.ispsum = ispsum
        self.t = t
        self.name = name
        self.lastw = None
        self.readers = []
        self.isdram = isdram
        self.base = base

    def __getitem__(self, idx):
        base = self.base if self.base is not None else (self.t.ap() if self.isdram else self.t[:])
        if idx == slice(None):
            return V(self, base)
        return V(self, base[idx])

    @property
    def v(self):
        return self[:]


class V:
    __slots__ = ("buf", "ap")

    def __init__(self, buf, ap):
        self.buf = buf
        self.ap = ap

    def __getitem__(self, idx):
        return V(self.buf, self.ap[idx])

    def rearrange(self, *a, **k):
        return V(self.buf, self.ap.rearrange(*a, **k))

    def pbc(self, n=128):
        return V(self.buf, self.ap.partition_broadcast(n))

    def bc(self, shape):
        return V(self.buf, self.ap.to_broadcast(shape))


class Prog:
    def __init__(self, nc):
        self.nc = nc
        self.es = ExitStack()
        self.scopes = [self.es]
        self.eng = {"pe": nc.tensor, "dve": nc.vector, "act": nc.scalar, "pool": nc.gpsimd, "sp": nc.sync}
        self.sems = {}
        self.cnt = {}
        self.known = {e: {} for e in self.eng}
        for e in ("pe", "dve", "act", "pool"):
            self.sems[e] = self.es.enter_context(nc.semaphore("s_" + e))
            self.cnt[e] = 0
        self.dma_ring = {}
        for q in ("sp", "pool", "act"):
            ring = []
            for i in range(N_DMA_SEMS):
                k = "d_%s_%d" % (q, i)
                self.sems[k] = self.es.enter_context(nc.semaphore(k))
                self.cnt[k] = 0
                ring.append(k)
            self.dma_ring[q] = [ring, 0]
        self.sems["cc"] = self.es.enter_context(nc.semaphore("s_cc"))
        self.cnt["cc"] = 0
        self.n_ins = 0
        self.uid = 0

    @contextmanager
    def scope(self):
        es = ExitStack()
        self.scopes.append(es)
        try:
            yield
        finally:
            self.barrier()
            self.scopes.pop()
            es.close()

    def sb(self, shape, dt=F32, name=None):
        self.uid += 1
        name = "%s_%d" % (name or "sb", self.uid)
        t = self.scopes[-1].enter_context(self.nc.sbuf_tensor(name, list(shape), dt))
        return Buf(t, name)

    def ps(self, shape, dt=F32, name=None):
        self.uid += 1
        name = "%s_%d" % (name or "ps", self.uid)
        t = self.scopes[-1].enter_context(self.nc.psum_tensor(name, list(shape), dt))
        return Buf(t, name, ispsum=True)

    def ps_multi(self, n, cols, dt=F32, name=None):
        per = (2048 // (4 if dt == F32 else 2)) // cols
        out = []
        while len(out) < n:
            k = min(per, n - len(out))
            self.uid += 1
            nm = "%s_%d" % (name or "psm", self.uid)
            t = self.scopes[-1].enter_context(self.nc.psum_tensor(nm, [128, per * cols], dt))
            bank = Buf(t, nm, ispsum=True)
            for j in range(k):
                out.append(V(bank, t[:][:, j * cols:(j + 1) * cols]))
        return out

    def dram(self, name, shape, dt=F32, kind="Internal", **kw):
        t = self.nc.dram_tensor(name, list(shape), dt, kind=kind, **kw)
        return Buf(t, name, isdram=True)

    def _wait(self, e, dep):
        if dep is None:
            return
        k, v = dep
        if self.known[e].get(k, 0) >= v:
            return
        self.eng[e].wait_ge(self.sems[k], v)
        self.known[e][k] = v

    def barrier(self):
        for e in self.eng:
            for k, c in self.cnt.items():
                if c > 0:
                    self._wait(e, (k, c))

    def _deps(self, e, reads, writes, pe=False):
        for r in reads:
            lw = r.buf.lastw
            if pe and lw is not None and lw[0] == "pe" and any(r.buf is w.buf for w in writes):
                continue
            self._wait(e, lw)
        for w in writes:
            lw = w.buf.lastw
            if not (pe and lw is not None and lw[0] == "pe"):
                self._wait(e, lw)
            for rd in w.buf.readers:
                self._wait(e, rd)

    def _commit(self, tok, reads, writes):
        for r in reads:
            rs = r.buf.readers
            rs.append(tok)
            if len(rs) > 16:
                best = {}
                for k, v in rs:
                    if best.get(k, 0) < v:
                        best[k] = v
                r.buf.readers = list(best.items())
        for w in writes:
            w.buf.lastw = tok
            w.buf.readers = []

    def op(self, e, fn, reads, writes):
        reads = [r for r in reads if isinstance(r, V)]
        if e != "pe":
            writes = list(writes) + [r for r in reads if r.buf.ispsum and not any(r.buf is w.buf for w in writes)]
        self._deps(e, reads, writes, pe=(e == "pe"))
        ins = fn()
        self.cnt[e] += 1
        ins.then_inc(self.sems[e], 1)
        self._commit((e, self.cnt[e]), reads, writes)
        self.n_ins += 1
        return ins

    def dma(self, out, in_, q="sp", **kw):
        ring, pos = self.dma_ring[q]
        k = ring[pos % N_DMA_SEMS]
        self.dma_ring[q][1] = pos + 1
        if self.cnt[k] > 0:
            self._wait(q, (k, self.cnt[k]))
        self._deps(q, [in_], [out])
        ins = self.eng[q].dma_start(out=out.ap, in_=in_.ap, **kw)
        self.cnt[k] += 16
        ins.then_inc(self.sems[k], 16)
        self._commit((k, self.cnt[k]), [in_], [out])
        self.n_ins += 1

    def collective(self, kind, out, in_):
        e = "pool"
        shp = list(in_.buf.t.shape)
        self.uid += 1
        xs = self.dram("ccin%d" % self.uid, shp, F32)
        ys = self.dram("ccout%d" % self.uid, shp, F32, addr_space="Shared")
        n0 = shp[0]
        for r0 in range(0, n0, 256):
            r1 = min(n0, r0 + 256)
            self.dma(xs[r0:r1, :], in_.buf[r0:r1, :], q="pool")
        self._deps(e, [xs[:]], [ys[:]])
        op = ALU.add if kind == "AllReduce" else ALU.bypass
        ins = self.nc.gpsimd.collective_compute(kind, op, replica_groups=[list(range(NCORES))],
                                               ins=[xs[:].ap], outs=[ys[:].ap])
        self.cnt["cc"] += 1
        ins.then_inc(self.sems["cc"], 1)
        self._commit(("cc", self.cnt["cc"]), [xs[:]], [ys[:]])
        self._wait(e, ("cc", self.cnt["cc"]))
        if not hasattr(self, "_dly"):
            self._dly = self.sb([128, 512], name="dly")
        for _ in range(96):
            self.memset(self._dly[:], 0.0, e="pool")
        for r0 in range(0, n0, 256):
            r1 = min(n0, r0 + 256)
            self.dma(out.buf[r0:r1, :], ys[r0:r1, :], q="pool")

    def finish(self):
        self.barrier()
        self.es.close()

    def mm(self, out, lhsT, rhs, start=True, stop=True):
        nc = self.nc
        return self.op("pe", lambda: nc.tensor.matmul(out.ap, lhsT.ap, rhs.ap, start=start, stop=stop),
                       [lhsT, rhs] + ([] if start else [out]), [out])

    def tr(self, out, in_, ident):
        nc = self.nc
        return self.op("pe", lambda: nc.tensor.transpose(out.ap, in_.ap, ident.ap), [in_, ident], [out])

    def act(self, out, in_, func, bias=None, scale=None, accum=None):
        nc = self.nc
        kw = {}
        rd = [in_]
        if bias is not None:
            kw["bias"] = bias.ap if isinstance(bias, V) else bias
            rd.append(bias)
        if scale is not None:
            kw["scale"] = scale.ap if isinstance(scale, V) else scale
            rd.append(scale)
        wr = [out]
        if accum is not None:
            kw["accum_out"] = accum.ap
            wr.append(accum)
        return self.op("act", lambda: nc.scalar.activation(out.ap, in_.ap, func, **kw), rd, wr)

    def ts(self, out, in0, s1, s2=None, op0=ALU.mult, op1=None, e="dve", accum=None):
        eng = self.eng[e]
        a1 = s1.ap if isinstance(s1, V) else s1
        a2 = s2.ap if isinstance(s2, V) else s2
        kw = {}
        wr = [out]
        if op1 is not None:
            kw["op1"] = op1
        if accum is not None:
            kw["accum_out"] = accum.ap
            wr.append(accum)
        return self.op(e, lambda: eng.tensor_scalar(out.ap, in0.ap, a1, a2, op0, **kw), [in0, s1, s2], wr)

    def tt(self, out, in0, in1, op, e="dve"):
        eng = self.eng[e]
        return self.op(e, lambda: eng.tensor_tensor(out.ap, in0.ap, in1.ap, op), [in0, in1], [out])

    def stt(self, out, in0, s, in1, op0, op1):
        nc = self.nc
        a = s.ap if isinstance(s, V) else s
        return self.op("dve", lambda: nc.vector.scalar_tensor_tensor(out.ap, in0.ap, a, in1.ap, op0, op1),
                       [in0, s, in1], [out])

    def copy(self, out, in_, e="dve"):
        eng = self.eng[e]
        if e == "act":
            return self.op(e, lambda: eng.copy(out.ap, in_.ap), [in_], [out])
        return self.op(e, lambda: eng.tensor_copy(out.ap, in_.ap), [in_], [out])

    def memset(self, out, val, e="pool"):
        eng = self.eng[e]
        return self.op(e, lambda: eng.memset(out.ap, val), [], [out])

    def recip(self, out, in_):
        nc = self.nc
        return self.op("dve", lambda: nc.vector.reciprocal(out.ap, in_.ap), [in_], [out])

    def max8(self, out, in_):
        nc = self.nc
        return self.op("dve", lambda: nc.vector.max(out.ap, in_.ap), [in_], [out])

    def match_replace(self, out, to_replace, values, imm):
        nc = self.nc
        return self.op("dve", lambda: nc.vector.match_replace(out.ap, to_replace.ap, values.ap, imm),
                       [to_replace, values], [out])


class Cfg:
    def __init__(self, NB=4, L=2048, LC=256, DE=1280):
        self.NB, self.L, self.LC, self.DE = NB, L, LC, DE
        self.D = 4096
        self.KC = 32
        self.TL = NB * L
        self.TC = NB * LC
        self.T = self.TL + self.TC
        self.NT = self.T // 128
        self.NTL = self.TL // 128
        self.NR = NB + 1
        self.E = 16
        self.capL = 2 * L // 16
        self.capC = 2 * LC // 16
        self.GW = 64
        assert self.T % NCORES == 0 and L % 128 == 0 and LC % 128 == 0
        assert self.capL % 8 == 0 and self.capC % 8 == 0
        self.TB = 256 if (L % 256 == 0 and LC % 256 == 0) else 128

    def tile_row(self, i):
        if i < self.NTL:
            return (i * 128) // self.L
        return self.NB

    def seqs(self):
        out = []
        for b in range(self.NB):
            out.append(("ctx", b, self.TL + b * self.LC, self.LC))
            out.append(("lat", b, b * self.L, self.L))
        return out


MODK = {"shift1": 0, "gm1": 1, "gate1": 2, "shift2": 3, "gm2": 4, "gate2": 5}


class Ctx:
    pass


def phase_gather_x(K):
    P, c = K.P, K.cfg
    rs = c.T // NCORES
    with P.scope():
        oh = P.sb([128, NCORES])
        P.dma(oh[:], K.onehot[:])
        xt = [P.sb([128, 4096]) for _ in range(2)]
        ot = [P.sb([128, 4096]) for _ in range(3)]
        n = 0
        for k in range((rs + 127) // 128):
            rn = min(128, rs - k * 128)
            x_ = xt[k % 2]
            P.dma(x_[0:rn, :], K.xs[k * 128:k * 128 + rn, :])
            for j in range(NCORES):
                o = ot[n % 3]
                n += 1
                P.ts(o[0:rn, :], x_[0:rn, :], oh[0:rn, j:j + 1], e=("dve" if j % 2 == 0 else "pool"))
                P.dma(K.xpad[j * rs + k * 128:j * rs + k * 128 + rn, :], o[0:rn, :], q=("sp" if j % 2 == 0 else "act"))
    P.collective("AllReduce", K.xres[:], K.xpad[:])


def phase_ada(K):
    P, c = K.P, K.cfg
    NR = c.NR
    with P.scope():
        condT = P.sb([128, 32, NR])
        P.dma(condT[:], K.condT[:])
        P.act(condT[:], condT[:], AF.Silu)
        ps = [P.ps([NR, 512]) for _ in range(6)]
        wbuf = [P.sb([128, 4, 3072]) for _ in range(2)]
        bias = P.sb([NR, 2, 3072])
        for l in range(2):
            P.dma(bias[:, l, :], K.adab[l, :].pbc(NR))
        res = P.sb([NR, 3072])
        resj = P.sb([NR, 3072])
        oh = P.sb([128, NCORES])
        P.dma(oh[:], K.onehot[:])
        for l in range(2):
            for kg in range(8):
                wb = wbuf[kg % 2]
                P.dma(wb[:], K.adaw[l, :, kg * 4:(kg + 1) * 4, :])
                for kk in range(4):
                    kc = kg * 4 + kk
                    for cb in range(6):
                        P.mm(ps[cb][:], condT[:, kc, :], wb[:, kk, cb * 512:(cb + 1) * 512],
                             start=(kc == 0), stop=(kc == 31))
            for cb in range(6):
                P.tt(res[:, cb * 512:(cb + 1) * 512], ps[cb][:], bias[:, l, cb * 512:(cb + 1) * 512], ALU.add)
            for j in range(NCORES):
                P.ts(resj[:], res[:], oh[0:NR, j:j + 1])
                P.dma(K.modpad[(j * 2 + l) * NR:(j * 2 + l + 1) * NR, :], resj[:], q="sp")
    P.collective("AllReduce", K.modall[:], K.modpad[:])


def phase_modrows(K):
    P, c = K.P, K.cfg
    NR = c.NR
    with P.scope():
        M = P.sb([NR, 6, 4096])
        nw = P.sb([NR, 2, 4096])
        for l in range(2):
            for k in range(6):
                src = K.modall[:].rearrange("(j l b) c -> l b j c", j=NCORES, l=2, b=NR)[l, :, :, k * 512:(k + 1) * 512]
                P.dma(M[:, k, :].rearrange("b (j c) -> b j c", j=NCORES), src)
            P.dma(nw[:, 0, :], K.nrm[l, :].pbc(NR))
            P.dma(nw[:, 1, :], K.nrm[2 + l, :].pbc(NR))
            P.stt(M[:, 1, :], M[:, 1, :], 1.0, nw[:, 0, :], ALU.add, ALU.mult)
            P.stt(M[:, 4, :], M[:, 4, :], 1.0, nw[:, 1, :], ALU.add, ALU.mult)
            P.dma(K.modrows[l * NR:(l + 1) * NR, :], M[:].rearrange("b k c -> b (k c)"), q="sp")


def rstd_from_ssq(P, rstd, ssq, tmp, n):
    P.ts(tmp, ssq, 1.0 / n, EPS, ALU.mult, ALU.add)
    P.act(tmp, tmp, AF.Sqrt)
    P.recip(rstd, tmp)


class NormMod:
    def __init__(self, K, l, which, add=None):
        P = K.P
        self.K, self.l, self.which, self.add = K, l, which, add
        self.gm = P.sb([128, 4096])
        self.sh = P.sb([128, 4096])
        self.st = P.sb([128, 4])
        if add is not None:
            self.gt = P.sb([128, 4096])
            self.yt = P.sb([128, 4096])
        self.row = None

    def load_row(self, row):
        K, P = self.K, self.K.P
        if row == self.row:
            return
        self.row = row
        r = self.l * K.cfg.NR + row
        kg = MODK["gm1"] if self.which == 1 else MODK["gm2"]
        ks = MODK["shift1"] if self.which == 1 else MODK["shift2"]
        P.dma(self.gm[:], K.modrows[r, kg * 4096:(kg + 1) * 4096].pbc(128))
        P.dma(self.sh[:], K.modrows[r, ks * 4096:(ks + 1) * 4096].pbc(128))
        if self.add is not None:
            la, ka = self.add[1], self.add[2]
            ra = la * K.cfg.NR + row
            P.dma(self.gt[:], K.modrows[ra, ka * 4096:(ka + 1) * 4096].pbc(128))

    def apply(self, hbf, xt, row, i):
        K, P = self.K, self.K.P
        self.load_row(row)
        st = self.st
        if self.add is not None:
            ybuf = self.add[0]
            P.dma(self.yt[:], ybuf[i * 128:(i + 1) * 128, :])
            P.tt(self.yt[:], self.yt[:], self.gt[:], ALU.mult, e="pool")
            P.tt(xt, xt, self.yt[:], ALU.add)
            P.dma(K.xres[i * 128:(i + 1) * 128, :], xt, q="act")
        P.act(hbf, xt, AF.Square, accum=st[:, 0:1])
        rstd_from_ssq(P, st[:, 2:3], st[:, 0:1], st[:, 1:2], 4096)
        P.stt(xt, xt, st[:, 2:3], self.gm[:], ALU.mult, ALU.mult)
        P.tt(xt, xt, self.sh[:], ALU.add, e="pool")
        P.copy(hbf, xt, e="pool")


def to_featmajor(K, hT, t0, hbf, psT, flip, fp32=False):
    P = K.P
    if fp32:
        for g in range(8):
            ps = psT[(flip + g) % 2]
            for q in range(4):
                kc = g * 4 + q
                P.tr(ps[:, q, :], hbf[:, kc * 128:(kc + 1) * 128], K.ident)
            P.copy(hT[:, g * 4:(g + 1) * 4, t0:t0 + 128], ps[:], e=("dve" if g % 2 == 0 else "act"))
        return
    for g in range(4):
        ps = psT[(flip + g) % 2]
        for q in range(8):
            kc = g * 8 + q
            P.tr(ps[:, q, :], hbf[:, kc * 128:(kc + 1) * 128], K.identb[:])
        if g % 2 == 0:
            P.copy(hT[:, g * 8:(g + 1) * 8, t0:t0 + 128], ps[:], e="dve")
        else:
            P.copy(hT[:, g * 8:(g + 1) * 8, t0:t0 + 128], ps[:], e="act")


def phase_proj(K, l, which, src, wfm, out_fm, wtm, out_tm, ntm, tiles=None, h_out=None, add=None, fp32=False):
    P, c = K.P, K.cfg
    TB = c.TB
    nblk = wfm.t.shape[0] if wfm is not None else 0
    tiles = list(range(c.NT)) if tiles is None else tiles
    with P.scope():
        nm = NormMod(K, l, which, add)
        xts = [P.sb([128, 4096]) for _ in range(2)]
        hbf = P.sb([128, 4096], BF16)
        assert not (fp32 and wfm is not None)
        hT = P.sb([128, 32, TB], F32 if fp32 else BF16)
        psT = [P.ps([128, 4, 128], F32) for _ in range(2)] if fp32 else [P.ps([128, 8, 128], BF16) for _ in range(2)]
        wb = [P.sb([128, 32, 128], BF16) for _ in range(2)]
        stg = [P.sb([128, 512]) for _ in range(2)]
        pso = [P.ps([128, 512]) for _ in range(2)]
        if wtm is not None:
            wt = P.sb([128, 32, ntm], F32 if fp32 else BF16)
            P.dma(wt[:], wtm[:], q=("sp" if fp32 else "pool"))
            pst = [P.ps([128, ntm]) for _ in range(2)]
            stt_ = [P.sb([128, ntm]) for _ in range(2)]
        nper = TB // 128
        assert len(tiles) % nper == 0
        cnt = 0
        for blk in range(len(tiles) // nper):
            tl = tiles[blk * nper:(blk + 1) * nper]
            for ti, i in enumerate(tl):
                xt = xts[cnt % 2]
                P.dma(xt[:], src[i * 128:(i + 1) * 128, :])
                nm.apply(hbf[:], xt[:], c.tile_row(i), i)
                if h_out is not None:
                    P.dma(h_out[i * 128:(i + 1) * 128, :], hbf[:], q="pool")
                to_featmajor(K, hT, ti * 128, xt if fp32 else hbf, psT, cnt, fp32)
                cnt += 1
            tok0 = tl[0] * 128
            assert all(tl[k] == tl[0] + k for k in range(nper))
            for j in range(nblk):
                w = wb[j % 2]
                P.dma(w[:], wfm[j, :, :, :], q="pool")
                for n0 in range(0, TB, 512):
                    nn = min(512, TB - n0)
                    ps = pso[(j + n0 // 512) % 2]
                    for kc in range(32):
                        P.mm(ps[:, 0:nn], w[:, kc, :], hT[:, kc, n0:n0 + nn], start=(kc == 0), stop=(kc == 31))
                    s = stg[(j + n0 // 512) % 2]
                    if j % 2 == 0:
                        P.copy(s[:, 0:nn], ps[:, 0:nn], e="dve")
                        P.dma(out_fm[j, :, tok0 + n0:tok0 + n0 + nn], s[:, 0:nn], q="sp")
                    else:
                        P.copy(s[:, 0:nn], ps[:, 0:nn], e="act")
                        P.dma(out_fm[j, :, tok0 + n0:tok0 + n0 + nn], s[:, 0:nn], q="act")
            if wtm is not None:
                for ti, i in enumerate(tl):
                    ps = pst[ti % 2]
                    for kc in range(32):
                        P.mm(ps[:], hT[:, kc, ti * 128:(ti + 1) * 128], wt[:, kc, :], start=(kc == 0), stop=(kc == 31))
                    s = stt_[ti % 2]
                    P.copy(s[:], ps[:], e="dve")
                    P.dma(out_tm[i * 128:(i + 1) * 128, :], s[:], q="sp")


def _blk(w):
    D, n = w.shape
    return np.ascontiguousarray(w.reshape(32, 128, n // 128, 128).transpose(2, 1, 0, 3))


def _kmaj(w):
    D, n = w.shape
    return np.ascontiguousarray(w.reshape(32, 128, n).transpose(1, 0, 2))


def make_consts():
    p = np.arange(128)[:, None]
    f = np.arange(128)[None, :]
    c = np.zeros((128, 6, 128), np.float32)
    c[:, 0, :] = (p == f)
    c[:, 1, :] = 1.0
    c[:, 2, :] = (p <= f)
    c[:, 3, :] = (p >= f)
    c[:, 4, :] = (p > f)
    c[:, 5, :] = (p < f)
    return c


def prep_inputs(cfg, inp, cores=None):
    c = cfg
    f32 = np.float32
    x = np.asarray(inp["x"], f32).reshape(c.TL, c.D)
    ctx = np.asarray(inp["ctx"], f32).reshape(c.TC, c.D)
    xall = np.concatenate([x, ctx], 0)
    cond = np.concatenate([np.asarray(inp["c"], f32), np.asarray(inp["c_ctx"], f32)[None]], 0)
    condT = np.ascontiguousarray(cond.T.reshape(32, 128, c.NR).transpose(1, 0, 2))
    nrm = np.stack([inp["norm_mix"][0], inp["norm_mix"][1], inp["norm_ffn"][0], inp["norm_ffn"][1],
                    inp["final_norm"]]).astype(f32)
    consts = make_consts()
    ada_w = np.asarray(inp["ada_w"])
    ada_b = np.asarray(inp["ada_b"])
    w_in = np.asarray(inp["w_in"])[0]
    GW = 3072
    maps = []
    rs = c.T // NCORES
    tpos = np.arange(c.L)
    inv = (10000.0 ** (-np.arange(32, dtype=np.float32) / 32)).astype(f32)
    ang_r = (tpos // c.GW).astype(f32)[:, None] * inv[None, :]
    ang_c = (tpos % c.GW).astype(f32)[:, None] * inv[None, :]
    ang = np.concatenate([ang_r, ang_r, ang_c, ang_c], -1)
    rope_c = np.ascontiguousarray(np.cos(ang).astype(f32).T)
    sgn = np.concatenate([-np.ones(32), np.ones(32), -np.ones(32), np.ones(32)]).astype(f32)
    rope_s = np.ascontiguousarray((np.sin(ang).astype(f32) * sgn[None, :]).T)
    for r in (range(NCORES) if cores is None else cores):
        m = {}
        m["xs"] = np.ascontiguousarray(xall[r * rs:(r + 1) * rs])
        m["condT"] = condT
        cols = np.concatenate([np.arange(k * 4096 + r * 512, k * 4096 + (r + 1) * 512) for k in range(6)])
        aw = ada_w[:, :, cols]
        m["adaw"] = np.ascontiguousarray(aw.reshape(2, 32, 128, 3072).transpose(0, 2, 1, 3))
        m["adab"] = np.ascontiguousarray(ada_b[:, cols])
        m["nrm"] = nrm
        m["consts"] = consts
        hs = np.arange(3 * r, 3 * r + 3)
        hc = (hs[:, None] * 128 + np.arange(128)[None, :]).reshape(-1)
        g = r // 2
        cols0 = np.concatenate([hc, GW + hc, 2 * GW + hc, 9312 + 1024 + hc, 9312 + g * 256 + np.arange(256)])
        m["win"] = _blk(w_in[:, cols0])
        stc = np.concatenate([9216 + k * 24 + hs for k in range(4)])
        m["wst"] = _kmaj(w_in[:, stc])
        cw = np.asarray(inp["conv_w"])[0]
        qkvc = np.concatenate([hc, GW + hc, 2 * GW + hc])
        m["convw"] = np.ascontiguousarray(cw[:, qkvc].T.reshape(9, 128, 5).transpose(1, 0, 2))
        gp = np.concatenate([np.asarray(inp["a_log_f"])[0][hs], np.asarray(inp["a_log_b"])[0][hs],
                             np.asarray(inp["dt_bias_f"])[0][hs], np.asarray(inp["dt_bias_b"])[0][hs]]).astype(f32)
        m["gprm"] = np.ascontiguousarray(np.broadcast_to(gp[None, :], (128, 12)))
        m["gnorm"] = np.ascontiguousarray(np.asarray(inp["gdn_norm"])[0].reshape(128, 1))
        half = r % 2
        pwg = np.asarray(inp["pool_w"])[0][g][:, half * 128:(half + 1) * 128]
        m["poolw"] = np.ascontiguousarray(pwg.reshape(2, 128, 128).transpose(1, 0, 2))
        m["poolsc"] = np.ascontiguousarray(np.asarray(inp["pool_scale"])[0][r * 128:(r + 1) * 128].reshape(128, 1))
        w_ = (2, 4, 8, 16)[g]
        tp = np.zeros(16, f32)
        tp[8 - w_ // 2:8 + w_ // 2] = 1.0
        m["ptaps"] = np.ascontiguousarray(np.broadcast_to(tp[None, :], (128, 16)))
        wo = np.asarray(inp["w_out_ab"])[0]
        rows = np.concatenate([r * 128 + np.arange(128), 1024 + hc])
        m["wo0"] = np.ascontiguousarray(wo[rows].reshape(4, 128, 4096).transpose(1, 0, 2))
        oh = np.zeros((128, NCORES), f32)
        oh[:, r] = 1.0
        m["onehot"] = oh
        wq = np.asarray(inp["w_qkv"])[0]
        hq = np.concatenate([(2 * r + hh_) * 256 + np.arange(256) for hh_ in range(2)])
        m["wqk"] = _blk(wq[:, np.concatenate([hq, c.D + hq])])
        m["wv"] = _kmaj(wq[:, 2 * c.D + hq])
        wc_ = np.asarray(inp["w_out_c"])[0]
        m["wo1"] = np.ascontiguousarray(wc_[hq].reshape(4, 128, 4096).transpose(1, 0, 2))
        m["lamv"] = np.ascontiguousarray(np.stack([np.asarray(inp[k_])[0] for k_ in ("lam_q1", "lam_k1", "lam_q2", "lam_k2")], 1).astype(f32))
        m["subw"] = np.ascontiguousarray(np.broadcast_to(np.asarray(inp["subln"])[0][None, :], (128, 256)).astype(f32))
        m["ropec"], m["ropes"] = rope_c, rope_s
        m["iota"] = np.ascontiguousarray(np.broadcast_to(np.arange(256, dtype=f32)[None, :], (128, 256)))
        perm = np.concatenate([[2 * r, 2 * r + 1], [e_ for e_ in range(16) if e_ not in (2 * r, 2 * r + 1)]])
        for l in range(2):
            m["rt%d" % l] = _kmaj(np.asarray(inp["router"])[l][:, perm])
            m["w1_%d" % l] = np.stack([_blk(np.asarray(inp["w1"])[l][e_]) for e_ in (2 * r, 2 * r + 1)])
            m["w3_%d" % l] = np.stack([_blk(np.asarray(inp["w3"])[l][e_]) for e_ in (2 * r, 2 * r + 1)])
            m["w2_%d" % l] = np.stack([np.ascontiguousarray(
                np.asarray(inp["w2"])[l][e_].reshape(c.DE // 128, 128, c.D).transpose(1, 0, 2)) for e_ in (2 * r, 2 * r + 1)])
        maps.append(m)
    return maps


def declare_inputs(K, P, cfg, maps0):
    for name, arr in maps0.items():
        setattr(K, name, P.dram(name, list(arr.shape), F32, kind="ExternalInput"))


def build(cfg, maps0, stop=None, outs=()):
    c = cfg
    nc = bass.Bass("TRN2", target_bir_lowering=False, num_devices=NCORES)
    P = Prog(nc)
    K = Ctx()
    K.P, K.cfg, K.nc = P, c, nc
    declare_inputs(K, P, c, maps0)
    NR = c.NR

    def scratch(name, shape, dt=F32, shared=False):
        kw = {"addr_space": "Shared"} if shared else {}
        b = P.dram(name, shape, dt, kind="Internal", **kw)
        setattr(K, name, b)
        return b

    K.out = P.dram("out", [c.TL // NCORES, c.D], F32, kind="ExternalOutput")

    nocc = "xres" in maps0
    if not nocc:
        scratch("xres", [c.T, c.D], shared=True)
        scratch("modall", [NCORES * 2 * NR, 3072], shared=True)
    if not nocc:
        scratch("xpad", [c.T, c.D])
        scratch("modpad", [NCORES * 2 * NR, 3072])
    scratch("modrows", [2 * NR, 6 * 4096])
    scratch("pfm0", [14, 128, c.T])
    scratch("pst0", [c.T, 12])
    scratch("qkvn", [9, 128, c.T])
    scratch("ymixT", [4, 128, c.T], BF16)
    scratch("ypart0", [c.T, c.D])
    if "yfull0" not in maps0:
        scratch("yfull0", [c.T, c.D], shared=True)
    scratch("logit0", [c.T, 16])
    scratch("h2_0", [c.T, c.D], BF16)
    scratch("fpart0", [c.T, c.D])
    for nm_ in ("ffull0", "yfull1", "ffull1"):
        if nm_ not in maps0:
            scratch(nm_, [c.T if nm_ == "ffull0" else c.TL, c.D], shared=True)
    scratch("pfm1", [8, 128, c.T])
    scratch("vtm1", [c.T, 512])
    scratch("attnT", [4, 128, c.TL], BF16)
    scratch("ypart1", [c.TL, c.D])
    scratch("logit1", [c.TL, 16])
    scratch("h2_1", [c.TL, c.D], BF16)
    scratch("fpart1", [c.TL, c.D])

    cst = P.sb([128, 6, 128])
    P.dma(cst[:], K.consts[:])
    K.cst = cst
    K.ident = cst[:, 0, :]
    K.ones = cst[:, 1, :]
    K.onecol = cst[:, 1, :]
    identb = P.sb([128, 128], BF16)
    P.copy(identb[:], cst[:, 0, :])
    K.identb = identb

    def done():
        for name in outs:
            src = getattr(K, name)
            shp = list(src.t.shape)
            dst = P.dram("dbg_" + name, shp, src.t.dtype, kind="ExternalOutput")
            n0 = shp[0]
            step = max(1, 256 // (int(np.prod(shp[1:-1])) if len(shp) > 2 else 1))
            for r0 in range(0, n0, step):
                r1 = min(n0, r0 + step)
                P.dma(dst[r0:r1], src[r0:r1])
        P.finish()
        return nc

    if not nocc:
        phase_gather_x(K)
        if stop == "gather":
            return done()
        phase_ada(K)
        if stop == "ada":
            return done()
    phase_modrows(K)
    if stop == "modrows":
        return done()
    phase_proj(K, 0, 1, K.xres, K.win, K.pfm0, K.wst, K.pst0, 12)
    if stop == "proj0":
        return done()
    phase_conv(K)
    if stop == "conv":
        return done()
    if stop != "pool":
        phase_gdn(K)
    if stop == "gdn":
        return done()
    phase_pool(K)
    if stop in ("mix0", "pool"):
        return done()
    phase_outproj(K, K.ymixT, K.wo0, K.ypart0, list(range(c.NT)))
    if stop == "out0":
        return done()
    if not nocc:
        P.collective("AllReduce", K.yfull0[:], K.ypart0[:])
    phase_proj(K, 0, 2, K.xres, None, None, K.rt0, K.logit0, 16, h_out=K.h2_0, add=(K.yfull0, 0, MODK["gate1"]),
               fp32=True)
    if stop == "res0":
        return done()
    sets0 = [(row0, ln, (c.capL if kind == "lat" else c.capC)) for (kind, b, row0, ln) in c.seqs()]
    phase_moe(K, 0, sets0, K.logit0, K.h2_0, K.w1_0, K.w3_0, K.w2_0, K.fpart0)
    if stop == "moe0":
        return done()
    if not nocc:
        P.collective("AllReduce", K.ffull0[:], K.fpart0[:])
    phase_proj(K, 1, 1, K.xres, K.wqk, K.pfm1, K.wv, K.vtm1, 512, add=(K.ffull0, 0, MODK["gate2"]))
    if stop == "proj1":
        return done()
    phase_attn(K)
    lat_tiles = list(range(c.NTL))
    phase_outproj(K, K.attnT, K.wo1, K.ypart1, lat_tiles)
    if stop == "out1":
        return done()
    if not nocc:
        P.collective("AllReduce", K.yfull1[:], K.ypart1[:])
    phase_proj(K, 1, 2, K.xres, None, None, K.rt1, K.logit1, 16, tiles=lat_tiles, h_out=K.h2_1,
               add=(K.yfull1, 1, MODK["gate1"]), fp32=True)
    sets1 = [(row0, ln, c.capL) for (kind, b, row0, ln) in c.seqs() if kind == "lat"]
    phase_moe(K, 1, sets1, K.logit1, K.h2_1, K.w1_1, K.w3_1, K.w2_1, K.fpart1)
    if stop == "moe1":
        return done()
    if not nocc:
        P.collective("AllReduce", K.ffull1[:], K.fpart1[:])
    phase_final(K, K.ffull1)
    return done()


def run_device(cfg, inp, stop=None, outs=(), trace=False, extra=None):
    maps = prep_inputs(cfg, inp)
    if extra is not None:
        for m in maps:
            m.update(extra)
            for k in ("xs", "condT", "adaw", "adab"):
                m.pop(k, None)
    nc = build(cfg, maps[0], stop=stop, outs=outs)
    res = run_bass_kernel_spmd(nc, maps, core_ids=list(range(NCORES)), trace=trace)
    return res


def phase_conv(K):
    P, c = K.P, K.cfg
    Lm = max(c.L, c.LC)
    with P.scope():
        cw = P.sb([128, 9, 5])
        P.dma(cw[:], K.convw[:])
        us = [P.sb([128, Lm + 4]) for _ in range(2)]
        y = P.sb([128, Lm])
        s = P.sb([128, Lm])
        sq = P.sb([128, Lm])
        rn = P.sb([128, Lm])
        psn = [P.ps([128, 512]) for _ in range(2)]
        for u in us:
            P.memset(u[:], 0.0)
        n = 0
        for (kind, b, row0, ln) in c.seqs():
            for blk in range(9):
                u = us[n % 2]
                n += 1
                if ln < Lm:
                    P.memset(u[:, ln + 2:ln + 4], 0.0)
                P.dma(u[:, 2:2 + ln], K.pfm0[blk, :, row0:row0 + ln])
                P.ts(y[:, 0:ln], u[:, 0:ln], cw[:, blk, 0:1])
                for j in range(1, 5):
                    P.stt(y[:, 0:ln], u[:, j:j + ln], cw[:, blk, j:j + 1], y[:, 0:ln], ALU.mult, ALU.add)
                P.act(s[:, 0:ln], y[:, 0:ln], AF.Silu)
                if blk < 6:
                    P.act(sq[:, 0:ln], s[:, 0:ln], AF.Square)
                    for c0 in range(0, ln, 512):
                        cn = min(512, ln - c0)
                        ps = psn[(c0 // 512) % 2]
                        P.mm(ps[:, 0:cn], K.ones, sq[:, c0:c0 + cn])
                        P.ts(rn[:, c0:c0 + cn], ps[:, 0:cn], EPS, None, ALU.add)
                    P.act(rn[:, 0:ln], rn[:, 0:ln], AF.Sqrt)
                    P.recip(rn[:, 0:ln], rn[:, 0:ln])
                    sc = (128 ** -0.5) if blk < 3 else 1.0
                    P.stt(s[:, 0:ln], s[:, 0:ln], sc, rn[:, 0:ln], ALU.mult, ALU.mult)
                P.dma(K.qkvn[blk, :, row0:row0 + ln], s[:, 0:ln], q="sp")


def phase_gates(K, G):
    P, c = K.P, K.cfg
    NT = c.NT
    with P.scope():
        prm = P.sb([128, 12])
        P.dma(prm[:], K.gprm[:])
        nega = P.sb([128, 6])
        P.act(nega[:], prm[:, 0:6], AF.Exp)
        P.ts(nega[:], nega[:], -1.0)
        st = P.sb([128, NT, 12])
        for i in range(NT):
            P.dma(st[:, i, :], K.pst0[i * 128:(i + 1) * 128, :])
        t = P.sb([128, NT, 6])
        for i in range(NT):
            P.tt(t[:, i, :], st[:, i, 0:6], prm[:, 6:12], ALU.add)
        P.act(t[:], t[:], AF.Exp)
        P.act(t[:], t[:], AF.Ln, bias=K.onecol[:, 0:1])
        for i in range(NT):
            P.tt(G["g"][:, i, :], t[:, i, :], nega[:], ALU.mult)
        P.act(G["beta"][:], st[:, :, 6:12], AF.Sigmoid)
        psa = P.ps([128, 6])
        psb = P.ps([128, 6])
        pst = P.ps([128, 6])
        for i in range(NT):
            P.mm(psa[:], K.cst[:, 2, :], G["g"][:, i, :])
            P.mm(psb[:], K.cst[:, 3, :], G["g"][:, i, :])
            P.mm(pst[:], K.ones, G["g"][:, i, :])
            P.copy(G["gc"][:, i, 0:3], psa[:, 0:3])
            P.copy(G["gc"][:, i, 3:6], psb[:, 3:6])
            P.copy(G["gt"][:, i, :], pst[:])
        P.act(G["eg"][:], G["gc"][:], AF.Exp)
        P.tt(t[:], G["gt"][:], G["gc"][:], ALU.subtract)
        P.act(G["kd"][:], t[:], AF.Exp)
        P.act(G["ge"][:], G["gt"][:], AF.Exp)
        P.ts(G["nb"][:], G["beta"][:], -1.0)
        P.tt(G["be"][:], G["beta"][:], G["eg"][:], ALU.mult)


def phase_gdn(K):
    P, c = K.P, K.cfg
    NT = c.NT
    cst = K.cst
    ident, ones = K.ident, K.ones
    with P.scope():
        G = {k: P.sb([128, NT, 6], name="G" + k) for k in ("g", "beta", "gc", "gt", "eg", "kd", "ge", "nb", "be")}
        phase_gates(K, G)
        if os.environ.get("GDBG") == "gates":
            return
        gn = P.sb([128, 1])
        P.dma(gn[:], K.gnorm[:])
        qk = [P.sb([128, 9, 128]) for _ in range(2)]
        nlt, nct = c.L // 128, c.LC // 128
        ntb = nlt + nct
        O = [P.sb([128, ntb, 128], name="O%d" % h) for h in range(3)]
        S = [[P.sb([128, 128], name="S%d%d" % (h, d)) for d in range(2)] for h in range(3)]
        psh = [P.ps_multi(4, 128) for h in range(3)]
        pscb = [P.ps_multi(4, 128) for h in range(3)]
        psc = [pscb[ci // 2][0:3] for ci in range(6)]
        chains = [(h, d) for h in range(3) for d in range(2)]
        T_ = {}
        for ci in range(6):
            for nm_ in ("gbc", "X1", "X2", "E1", "E2", "M", "MT", "Pw", "PwT", "Q", "at", "ru", "rw", "kd", "u", "wT",
                        "vn", "o1"):
                T_[(ci, nm_)] = P.sb([128, 128], name="%s%d" % (nm_, ci))
        zt = P.sb([128, 128])
        sz = P.sb([128, 128])
        on = P.sb([128, 128])
        yb = P.sb([128, 128], BF16)
        stt_ = P.sb([128, 4])
        pso = pscb[0][3]
        cnt = 0
        for b in range(c.NB):
            ctiles = [c.NTL + b * nct + n for n in range(nct)]
            ltiles = [b * nlt + n for n in range(nlt)]
            oidx = {t: n for n, t in enumerate(ctiles + ltiles)}
            for h in range(3):
                for d in range(2):
                    P.memset(S[h][d][:], 0.0)
            order_f = ctiles + ltiles
            order_b = ctiles[::-1] + ltiles[::-1]
            first_done = set()
            for step in range(ntb):
                for d, tile in ((0, order_f[step]), (1, order_b[step])):
                    q = qk[cnt % 2]
                    cnt += 1
                    P.dma(q[:], K.qkvn[:, :, tile * 128:(tile + 1) * 128].rearrange("k p t -> p k t"))
                    for h in range(3):
                        kT, qT, vT = q[:, 3 + h, :], q[:, h, :], q[:, 6 + h, :]
                        P.mm(psh[h][0][:], kT, kT)
                        P.mm(psh[h][1][:], kT, qT)
                        P.tr(psh[h][2][:], kT, ident)
                        P.tr(psh[h][3][:], vT, ident)
                    if os.environ.get("GDBG") == "shared":
                        return
                    tri = cst[:, 2, :] if d == 0 else cst[:, 3, :]
                    m1 = cst[:, 4, :] if d == 0 else cst[:, 5, :]
                    m2 = cst[:, 2, :] if d == 0 else cst[:, 3, :]

                    def t(h, n):
                        return T_[(h * 2 + d, n)]

                    def col(name, h):
                        return G[name][:, tile, d * 3 + h:d * 3 + h + 1]
                    for h in range(3):
                        P.ts(t(h, "gbc")[:], ones, col("g", h))
                    for h in range(3):
                        P.mm(psc[h * 2 + d][0][:], t(h, "gbc")[:], tri)
                    for h in range(3):
                        g0 = psc[h * 2 + d][0]
                        P.ts(t(h, "X1")[:], g0[:], col("gc", h), 0.0, ALU.subtract, ALU.max)
                        P.ts(t(h, "X2")[:], g0[:], col("gc", h), 0.0, ALU.subtract, ALU.min)
                    for h in range(3):
                        P.act(t(h, "E1")[:], t(h, "X1")[:], AF.Exp, scale=-1.0)
                        P.act(t(h, "E2")[:], t(h, "X2")[:], AF.Exp)
                    for h in range(3):
                        P.tt(t(h, "E1")[:], t(h, "E1")[:], m1, ALU.mult, e="pool")
                        P.tt(t(h, "E2")[:], t(h, "E2")[:], m2, ALU.mult, e="pool")
                    for h in range(3):
                        P.stt(t(h, "M")[:], psh[h][0][:], col("nb", h), t(h, "E1")[:], ALU.mult, ALU.mult)
                        P.tt(t(h, "at")[:], psh[h][1][:], t(h, "E2")[:], ALU.mult)
                        P.ts(t(h, "ru")[:], psh[h][3][:], col("beta", h))
                        P.ts(t(h, "rw")[:], psh[h][2][:], col("be", h))
                        P.ts(t(h, "kd")[:], psh[h][2][:], col("kd", h))
                    if os.environ.get("GDBG") == "pre":
                        return
                    for h in range(3):
                        P.tr(psc[h * 2 + d][1][:], t(h, "M")[:], ident)
                    for h in range(3):
                        P.copy(t(h, "MT")[:], psc[h * 2 + d][1][:], e="act")
                        P.tt(t(h, "Q")[:], psc[h * 2 + d][1][:], ident, ALU.add)
                    if os.environ.get("GDBG") == "mt":
                        return
                    cur, nxt = ("M", "MT"), ("Pw", "PwT")
                    for lvl in range(1, 7):
                        if os.environ.get("GDBG") == "l%d" % lvl:
                            return
                        for h in range(3):
                            P.mm(psc[h * 2 + d][0][:], t(h, cur[1])[:], t(h, cur[0])[:])
                            if lvl < 6:
                                P.mm(psc[h * 2 + d][1][:], t(h, cur[0])[:], t(h, cur[1])[:])
                        if os.environ.get("GDBG") == "a%d" % lvl:
                            return
                        for h in range(3):
                            P.copy(t(h, nxt[0])[:], psc[h * 2 + d][0][:], e="act")
                            if lvl < 6:
                                P.copy(t(h, nxt[1])[:], psc[h * 2 + d][1][:], e="dve")
                        if os.environ.get("GDBG") == "b%d" % lvl:
                            return
                        for h in range(3):
                            P.mm(psc[h * 2 + d][2][:], t(h, nxt[0])[:], t(h, "Q")[:])
                        for h in range(3):
                            P.tt(t(h, "Q")[:], t(h, "Q")[:], psc[h * 2 + d][2][:], ALU.add)
                        cur, nxt = nxt, cur
                    if os.environ.get("GDBG") == "neu":
                        return
                    for h in range(3):
                        P.mm(psc[h * 2 + d][0][:], t(h, "Q")[:], t(h, "ru")[:])
                        P.mm(psc[h * 2 + d][1][:], t(h, "rw")[:], t(h, "Q")[:])
                    for h in range(3):
                        P.copy(t(h, "u")[:], psc[h * 2 + d][0][:], e="act")
                        P.copy(t(h, "wT")[:], psc[h * 2 + d][1][:], e="dve")
                    for h in range(3):
                        P.mm(psc[h * 2 + d][2][:], t(h, "wT")[:], S[h][d][:])
                        P.mm(psc[h * 2 + d][0][:], q[:, h, :], S[h][d][:])
                    for h in range(3):
                        P.tt(t(h, "vn")[:], t(h, "u")[:], psc[h * 2 + d][2][:], ALU.subtract)
                        P.act(t(h, "o1")[:], psc[h * 2 + d][0][:], AF.Copy, scale=col("eg", h))
                    for h in range(3):
                        P.mm(psc[h * 2 + d][1][:], t(h, "at")[:], t(h, "vn")[:])
                        P.mm(psc[h * 2 + d][2][:], t(h, "kd")[:], t(h, "vn")[:])
                    for h in range(3):
                        ot = O[h][:, oidx[tile], :]
                        if (h, tile) not in first_done:
                            P.tt(ot, t(h, "o1")[:], psc[h * 2 + d][1][:], ALU.add)
                            first_done.add((h, tile))
                        else:
                            P.tt(t(h, "o1")[:], t(h, "o1")[:], psc[h * 2 + d][1][:], ALU.add)
                            P.tt(ot, ot, t(h, "o1")[:], ALU.add, e="pool")
                        P.stt(S[h][d][:], S[h][d][:], col("ge", h), psc[h * 2 + d][2][:], ALU.mult, ALU.add)
            if os.environ.get("GDBG") == "scan":
                return
            for h in range(3):
                for tile in ctiles + ltiles:
                    ot = O[h][:, oidx[tile], :]
                    P.act(on[:], ot, AF.Square, accum=stt_[:, 0:1])
                    rstd_from_ssq(P, stt_[:, 2:3], stt_[:, 0:1], stt_[:, 1:2], 128)
                    P.ts(on[:], ot, stt_[:, 2:3])
                    P.tr(pso[:], on[:], ident)
                    P.dma(zt[:], K.pfm0[9 + h, :, tile * 128:(tile + 1) * 128])
                    P.act(sz[:], zt[:], AF.Silu)
                    P.stt(yb[:], pso[:], gn[:, 0:1], sz[:], ALU.mult, ALU.mult)
                    P.dma(K.ymixT[1 + h, :, tile * 128:(tile + 1) * 128], yb[:], q="sp")


def phase_pool(K):
    P, c = K.P, K.cfg
    Lm = max(c.L, c.LC)
    with P.scope():
        taps = P.sb([128, 16])
        P.dma(taps[:], K.ptaps[:])
        pw32 = P.sb([128, 2, 128])
        P.dma(pw32[:], K.poolw[:])
        psc_ = P.sb([128, 1])
        P.dma(psc_[:], K.poolsc[:])
        u = P.sb([128, 2, Lm + 16])
        acc = P.sb([128, Lm])
        onesp = P.sb([128, Lm + 16])
        rc = {}
        dd = P.sb([128, 2, Lm])
        yb = P.sb([128, 512], BF16)
        ps = [P.ps([128, 512]) for _ in range(2)]
        P.memset(u[:], 0.0)
        for ln in sorted({c.L, c.LC}):
            P.memset(onesp[:], 0.0)
            P.memset(onesp[:, 8:8 + ln], 1.0)
            r = P.sb([128, ln], name="rc%d" % ln)
            P.ts(r[:], onesp[:, 0:ln], taps[:, 0:1])
            for j in range(1, 16):
                P.stt(r[:], onesp[:, j:j + ln], taps[:, j:j + 1], r[:], ALU.mult, ALU.add)
            P.recip(r[:], r[:])
            rc[ln] = r
        n = 0
        for (kind, b, row0, ln) in c.seqs():
            if ln < Lm:
                P.memset(u[:, :, 8 + ln:8 + ln + 8], 0.0)
            for blk in range(2):
                P.dma(u[:, blk, 8:8 + ln], K.pfm0[12 + blk, :, row0:row0 + ln])
            for blk in range(2):
                P.ts(acc[:, 0:ln], u[:, blk, 0:ln], taps[:, 0:1])
                for j in range(1, 16):
                    P.stt(acc[:, 0:ln], u[:, blk, j:j + ln], taps[:, j:j + 1], acc[:, 0:ln], ALU.mult, ALU.add)
                P.tt(acc[:, 0:ln], acc[:, 0:ln], rc[ln][:], ALU.mult)
                P.tt(dd[:, blk, 0:ln], acc[:, 0:ln], u[:, blk, 8:8 + ln], ALU.subtract)
            for c0 in range(0, ln, 512):
                cn = min(512, ln - c0)
                p = ps[n % 2]
                n += 1
                for blk in range(2):
                    P.mm(p[:, 0:cn], pw32[:, blk, :], dd[:, blk, c0:c0 + cn], start=(blk == 0), stop=(blk == 1))
                P.ts(yb[:, 0:cn], p[:, 0:cn], psc_[:, 0:1])
                P.dma(K.ymixT[0, :, row0 + c0:row0 + c0 + cn], yb[:, 0:cn], q="sp")


def phase_outproj(K, srcT, w, ypart, tiles):
    P, c = K.P, K.cfg
    with P.scope():
        wb = P.sb([128, 4, 4096], BF16)
        P.dma(wb[:], w[:], q="pool")
        a = [P.sb([128, 4, 128], BF16) for _ in range(2)]
        o = [P.sb([128, 4096]) for _ in range(2)]
        ps = [P.ps([128, 512]) for _ in range(4)]
        for n, i in enumerate(tiles):
            at = a[n % 2]
            P.dma(at[:], srcT[:, :, i * 128:(i + 1) * 128].rearrange("k p t -> p k t"))
            ot = o[n % 2]
            for g in range(8):
                p = ps[g % 4]
                for blk in range(4):
                    P.mm(p[:], at[:, blk, :], wb[:, blk, g * 512:(g + 1) * 512], start=(blk == 0), stop=(blk == 3))
                P.copy(ot[:, g * 512:(g + 1) * 512], p[:], e=("dve" if g % 2 == 0 else "act"))
            P.dma(ypart[i * 128:(i + 1) * 128, :], ot[:], q="sp")


def phase_moe(K, l, sets, logit, h2, w1, w3, w2, fpart):
    P, c = K.P, K.cfg
    cst = K.cst
    DE = c.DE
    NF = DE // 128
    caps = [cp for (_, _, cp) in sets]
    off = []
    pos_ = 0
    for cp in caps:
        if cp >= 128:
            pos_ = (pos_ + 127) // 128 * 128
        else:
            pos_ = (pos_ + 31) // 32 * 32
            if (pos_ % 128) + cp > 96:
                pos_ = (pos_ + 127) // 128 * 128
        off.append(pos_)
        pos_ += cp
    NCAP = (pos_ + 31) // 32 * 32
    Lm = max(ln for (_, ln, _) in sets)
    capm = max(caps)
    nst = (NCAP + 127) // 128
    ntl = {}
    with P.scope():
        maskT = P.sb([128, c.NT, 2])
        gateT = P.sb([128, c.NT, 2])
        posT = P.sb([128, c.NT, 2])
        iot = P.sb([128, capm])
        P.dma(iot[:], K.iota[:, 0:capm])
        with P.scope():
            lg = P.sb([128, 16])
            ex = P.sb([128, 16])
            sm = P.sb([128, 4])
            affT = P.sb([16, Lm])
            work = P.sb([16, Lm])
            m8 = P.sb([16, 8])
            mk = P.sb([16, Lm])
            gt_ = P.sb([16, Lm])
            pst = P.ps([16, 128])
            ps2 = P.ps([128, 2])
            ps3 = P.ps([128, 2])
            tot = P.sb([128, 2])
            for si, (row0, ln, cap) in enumerate(sets):
                nt_ = ln // 128
                for n in range(nt_):
                    i = row0 // 128 + n
                    P.dma(lg[:], logit[i * 128:(i + 1) * 128, :])
                    P.op("dve", lambda: K.nc.vector.reduce_max(sm[:, 0:1].ap, lg[:].ap, axis=AX.X), [lg[:]], [sm[:, 0:1]])
                    P.ts(sm[:, 1:2], sm[:, 0:1], -1.0)
                    P.act(ex[:], lg[:], AF.Exp, bias=sm[:, 1:2], accum=sm[:, 2:3])
                    P.recip(sm[:, 3:4], sm[:, 2:3])
                    P.ts(ex[:], ex[:], sm[:, 3:4])
                    P.tr(pst[:], ex[:], K.ident)
                    P.copy(affT[:, n * 128:(n + 1) * 128], pst[:])
                src = affT
                for it in range(cap // 8):
                    P.max8(m8[:], src[:, 0:ln])
                    if it < cap // 8 - 1:
                        P.match_replace(work[:, 0:ln], m8[:], src[:, 0:ln], -1.0)
                        src = work
                P.ts(mk[:, 0:ln], affT[:, 0:ln], m8[:, 7:8], None, ALU.is_ge)
                P.tt(gt_[:, 0:ln], mk[:, 0:ln], affT[:, 0:ln], ALU.mult)
                P.memset(tot[:], 0.0)
                for n in range(nt_):
                    i = row0 // 128 + n
                    P.tr(ps2[:], mk[0:2, n * 128:(n + 1) * 128], K.ident[0:2, 0:2])
                    P.copy(maskT[:, i, :], ps2[:])
                    P.tr(ps3[:], gt_[0:2, n * 128:(n + 1) * 128], K.ident[0:2, 0:2])
                    P.copy(gateT[:, i, :], ps3[:])
                    P.mm(ps2[:], cst[:, 5, :], maskT[:, i, :])
                    P.tt(posT[:, i, :], ps2[:], tot[:], ALU.add)
                    P.mm(ps3[:], K.ones, maskT[:, i, :])
                    P.tt(tot[:], tot[:], ps3[:], ALU.add)
        for e in range(2):
            with P.scope():
                hid = P.sb([128, NF, NCAP], BF16)
                selg = P.sb([128, c.NT, capm], BF16)
                with P.scope():
                    xeT = P.sb([128, 32, NCAP], BF16)
                    with P.scope():
                        sel = P.sb([128, capm], BF16)
                        ht = [P.sb([128, 512], BF16) for _ in range(3)]
                        psg = [P.ps([128, 512]) for _ in range(4)]
                        P.memset(xeT[:], 0.0)
                        hc = 0
                        for si, (row0, ln, cap) in enumerate(sets):
                            nt_ = ln // 128
                            for n in range(nt_):
                                i = row0 // 128 + n
                                P.ts(selg[:, i, 0:cap], iot[:, 0:cap], posT[:, i, e:e + 1], gateT[:, i, e:e + 1],
                                     ALU.is_equal, ALU.mult)
                            for kg in range(8):
                                for n in range(nt_):
                                    i = row0 // 128 + n
                                    P.ts(sel[:, 0:cap], iot[:, 0:cap], posT[:, i, e:e + 1], maskT[:, i, e:e + 1],
                                         ALU.is_equal, ALU.mult)
                                    h = ht[hc % 3]
                                    hc += 1
                                    P.dma(h[:], h2[i * 128:(i + 1) * 128, kg * 512:(kg + 1) * 512])
                                    for q in range(4):
                                        P.mm(psg[q][:, 0:cap], h[:, q * 128:(q + 1) * 128], sel[:, 0:cap],
                                             start=(n == 0), stop=(n == nt_ - 1))
                                for q in range(4):
                                    P.copy(xeT[:, kg * 4 + q, off[si]:off[si] + cap], psg[q][:, 0:cap],
                                           e=("dve" if q % 2 == 0 else "act"))
                    with P.scope():
                        wa = [P.sb([128, 32, 128], BF16) for _ in range(2)]
                        wc = [P.sb([128, 32, 128], BF16) for _ in range(2)]
                        nch = (NCAP + 511) // 512
                        ps1 = [P.ps([128, 512]) for _ in range(nch)]
                        ps3_ = [P.ps([128, 512]) for _ in range(nch)]
                        sg = P.sb([128, 512])
                        for fc in range(NF):
                            a, b3 = wa[fc % 2], wc[fc % 2]
                            P.dma(a[:], w1[e, fc, :, :, :], q="pool")
                            P.dma(b3[:], w3[e, fc, :, :, :], q="pool")
                            for ch in range(nch):
                                c0 = ch * 512
                                cn = min(512, NCAP - c0)
                                for kc in range(32):
                                    P.mm(ps1[ch][:, 0:cn], a[:, kc, :], xeT[:, kc, c0:c0 + cn], start=(kc == 0), stop=(kc == 31))
                                for kc in range(32):
                                    P.mm(ps3_[ch][:, 0:cn], b3[:, kc, :], xeT[:, kc, c0:c0 + cn], start=(kc == 0), stop=(kc == 31))
                                P.act(sg[:, 0:cn], ps1[ch][:, 0:cn], AF.Silu)
                                P.tt(hid[:, fc, c0:c0 + cn], sg[:, 0:cn], ps3_[ch][:, 0:cn], ALU.mult)
                for half in range(2):
                  with P.scope():
                    ye = P.sb([128, nst, 2048], BF16)
                    w2s = [P.sb([128, NF, 512], BF16) for _ in range(2)]
                    psy = [P.ps([128, 512]) for _ in range(2)]
                    for g in range(4):
                        w = w2s[g % 2]
                        gg = half * 4 + g
                        P.dma(w[:], w2[e, :, :, gg * 512:(gg + 1) * 512], q="pool")
                        for st_ in range(nst):
                            s0 = st_ * 128
                            sn = min(128, NCAP - s0)
                            p = psy[st_ % 2]
                            for fc in range(NF):
                                P.mm(p[0:sn, :], hid[:, fc, s0:s0 + sn], w[:, fc, :], start=(fc == 0), stop=(fc == NF - 1))
                            P.copy(ye[0:sn, st_, g * 512:(g + 1) * 512], p[0:sn, :], e=("dve" if st_ % 2 == 0 else "act"))
                    sgT = [P.sb([128, 2, 128], BF16) for _ in range(2)]
                    pT = P.ps([128, 2, 128], BF16)
                    pso = [P.ps([128, 512]) for _ in range(4)]
                    ot = [P.sb([128, 2048]) for _ in range(2)]
                    prev = P.sb([128, 2048])
                    cnt = 0
                    for si, (row0, ln, cap) in enumerate(sets):
                        nt_ = ln // 128
                        pieces = []
                        s_ = off[si]
                        while s_ < off[si] + cap:
                            st_ = s_ // 128
                            n_ = min((st_ + 1) * 128, off[si] + cap) - s_
                            pieces.append((st_, s_ - st_ * 128, s_ - off[si], n_))
                            s_ += n_
                        for n in range(nt_):
                            i = row0 // 128 + n
                            sT = sgT[cnt % 2]
                            o = ot[cnt % 2]
                            cnt += 1
                            for pi, (st_, p0, l0, n_) in enumerate(pieces):
                                P.tr(pT[p0:p0 + n_, pi, :], selg[:, i, l0:l0 + n_], K.identb[:])
                                P.copy(sT[p0:p0 + n_, pi, :], pT[p0:p0 + n_, pi, :])
                            if e == 1:
                                P.dma(prev[:], fpart[i * 128:(i + 1) * 128, half * 2048:(half + 1) * 2048])
                            for g in range(4):
                                p = pso[g]
                                for pi, (st_, p0, l0, n_) in enumerate(pieces):
                                    P.mm(p[:], sT[p0:p0 + n_, pi, :], ye[p0:p0 + n_, st_, g * 512:(g + 1) * 512],
                                         start=(pi == 0), stop=(pi == len(pieces) - 1))
                                if e == 0:
                                    P.copy(o[:, g * 512:(g + 1) * 512], p[:], e=("dve" if g % 2 == 0 else "act"))
                                else:
                                    P.tt(o[:, g * 512:(g + 1) * 512], p[:], prev[:, g * 512:(g + 1) * 512], ALU.add)
                            P.dma(fpart[i * 128:(i + 1) * 128, half * 2048:(half + 1) * 2048], o[:], q="sp")


LAMBDA_INIT1 = 0.8 - 0.6 * math.exp(-0.3 * 1)


def phase_attn(K):
    P, c = K.P, K.cfg
    L, LC = c.L, c.LC
    NK = (L + LC) // 128
    QB = min(512, L)
    nqt = QB // 128
    with P.scope():
        cosT = P.sb([128, L])
        sinT = P.sb([128, L])
        P.dma(cosT[:], K.ropec[:])
        P.dma(sinT[:], K.ropes[:])
        lv = P.sb([128, 4])
        P.dma(lv[:], K.lamv[:])
        pr = P.sb([128, 2])
        P.tt(pr[:, 0:1], lv[:, 0:1], lv[:, 1:2], ALU.mult)
        P.tt(pr[:, 1:2], lv[:, 2:3], lv[:, 3:4], ALU.mult)
        psl = P.ps([128, 2])
        P.mm(psl[:], K.ones, pr[:])
        ee = P.sb([128, 2])
        P.act(ee[:], psl[:], AF.Exp)
        nlam = P.sb([128, 1])
        P.tt(nlam[:], ee[:, 1:2], ee[:, 0:1], ALU.subtract)
        P.ts(nlam[:], nlam[:], -LAMBDA_INIT1, None, ALU.add)
        subw = P.sb([128, 256])
        P.dma(subw[:], K.subw[:])
        raw = P.sb([128, L + LC])
        prm = P.sb([128, L])
        tmp = P.sb([128, L])
        qTb = P.sb([128, L], BF16)
        kTb = P.sb([128, L + LC], BF16)
        vst = P.sb([128, 256])
        vaug = P.sb([128, NK, 257], BF16)
        E = [P.sb([128, QB], BF16) for _ in range(2)]
        pss = [P.ps([128, 512]) for _ in range(2)]
        pso = [P.ps([128, 257]) for _ in range(4)]
        o0 = P.sb([128, L // 128, 256])
        o1 = P.sb([128, 256])
        on = P.sb([128, 256])
        st = P.sb([128, 4])
        pT = P.ps([128, 2, 128])
        ob = P.sb([128, 2, 128], BF16)
        perm = [(0, 32), (32, 0), (64, 96), (96, 64)]

        def load_rope(dst_bf, blk, row0, n, do_rope, p0=0):
            P.dma(raw[:, 0:n], K.pfm1[blk, :, row0:row0 + n])
            if do_rope:
                for (d0, s0) in perm:
                    P.dma(prm[d0:d0 + 32, 0:n], K.pfm1[blk, s0:s0 + 32, row0:row0 + n])
                P.tt(tmp[:, 0:n], raw[:, 0:n], cosT[:, p0:p0 + n], ALU.mult)
                P.tt(prm[:, 0:n], prm[:, 0:n], sinT[:, p0:p0 + n], ALU.mult, e="pool")
                P.tt(dst_bf, tmp[:, 0:n], prm[:, 0:n], ALU.add)
            else:
                P.copy(dst_bf, raw[:, 0:n], e="pool")

        for b in range(c.NB):
            lrow, crow = b * L, c.TL + b * LC
            for hh in range(2):
                P.memset(vaug[:, :, 256:257], 1.0)
                for kt in range(NK):
                    r0 = lrow + kt * 128 if kt < L // 128 else crow + (kt - L // 128) * 128
                    P.dma(vst[:], K.vtm1[r0:r0 + 128, hh * 256:(hh + 1) * 256])
                    P.copy(vaug[:, kt, 0:256], vst[:], e="pool")
                for m in range(2):
                    load_rope(kTb[:, 0:L], 4 + hh * 2 + m, lrow, L, True)
                    load_rope(kTb[:, L:L + LC], 4 + hh * 2 + m, crow, LC, False)
                    for q0 in range(0, L, QB):
                        load_rope(qTb[:, 0:QB], hh * 2 + m, lrow + q0, QB, True, p0=q0)
                        for kt in range(NK):
                            ps = pss[kt % 2]
                            P.mm(ps[:, 0:QB], kTb[:, kt * 128:(kt + 1) * 128], qTb[:, 0:QB])
                            e_ = E[kt % 2]
                            P.act(e_[:], ps[:, 0:QB], AF.Exp, scale=128 ** -0.5)
                            for qt in range(nqt):
                                P.mm(pso[qt][:], e_[:, qt * 128:(qt + 1) * 128], vaug[:, kt, :],
                                     start=(kt == 0), stop=(kt == NK - 1))
                        for qt in range(nqt):
                            qi = q0 // 128 + qt
                            P.recip(st[:, 0:1], pso[qt][:, 256:257])
                            if m == 0:
                                P.ts(o0[:, qi, :], pso[qt][:, 0:256], st[:, 0:1])
                            else:
                                P.ts(o1[:], pso[qt][:, 0:256], st[:, 0:1])
                                P.stt(o1[:], o1[:], nlam[:, 0:1], o0[:, qi, :], ALU.mult, ALU.add)
                                P.act(on[:], o1[:], AF.Square, accum=st[:, 1:2])
                                rstd_from_ssq(P, st[:, 3:4], st[:, 1:2], st[:, 2:3], 256)
                                P.stt(on[:], o1[:], st[:, 3:4], subw[:], ALU.mult, ALU.mult)
                                for blk in range(2):
                                    P.tr(pT[:, blk, :], on[:, blk * 128:(blk + 1) * 128], K.ident)
                                P.act(ob[:], pT[:], AF.Copy, scale=(1.0 - LAMBDA_INIT1))
                                t0 = lrow + qi * 128
                                P.dma(K.attnT[hh * 2:hh * 2 + 2, :, t0:t0 + 128].rearrange("k p t -> p k t"), ob[:], q="sp")


def phase_final(K, fbuf):
    P, c = K.P, K.cfg
    rs = c.TL // NCORES
    with P.scope():
        oh = P.sb([128, NCORES])
        P.dma(oh[:], K.onehot[:])
        fw = P.sb([128, 4096])
        P.dma(fw[:], K.nrm[4, :].pbc(128))
        gt = P.sb([128, 4096])
        xt = [P.sb([128, 4096]) for _ in range(2)]
        yt = [P.sb([128, 4096]) for _ in range(2)]
        junk = P.sb([128, 4096], BF16)
        st = P.sb([128, 4])
        nt_ = (rs + 127) // 128
        acc = P.sb([128, 4096], name="facc")
        row = None
        n = 0
        for k in range(nt_):
            rn = min(128, rs - k * 128)
            P.memset(acc[:], 0.0)
            for j in range(NCORES):
                r0 = j * rs + k * 128
                b = r0 // c.L
                assert (r0 + rn - 1) // c.L == b
                if b != row:
                    row = b
                    P.dma(gt[:], K.modrows[1 * c.NR + b, MODK["gate2"] * 4096:(MODK["gate2"] + 1) * 4096].pbc(128))
                x_, y_ = xt[n % 2], yt[n % 2]
                n += 1
                P.dma(x_[0:rn, :], K.xres[r0:r0 + rn, :])
                P.dma(y_[0:rn, :], fbuf[r0:r0 + rn, :])
                P.tt(y_[0:rn, :], y_[0:rn, :], gt[0:rn, :], ALU.mult, e="pool")
                P.tt(x_[0:rn, :], x_[0:rn, :], y_[0:rn, :], ALU.add)
                P.act(junk[0:rn, :], x_[0:rn, :], AF.Square, accum=st[0:rn, 0:1])
                rstd_from_ssq(P, st[0:rn, 2:3], st[0:rn, 0:1], st[0:rn, 1:2], 4096)
                P.stt(x_[0:rn, :], x_[0:rn, :], st[0:rn, 2:3], fw[0:rn, :], ALU.mult, ALU.mult)
                P.stt(acc[0:rn, :], x_[0:rn, :], oh[0:rn, j:j + 1], acc[0:rn, :], ALU.mult, ALU.add)
            P.dma(K.out[k * 128:k * 128 + rn, :], acc[0:rn, :], q="sp")


def kernel(**inputs):
    cfg = Cfg()
    maps = prep_inputs(cfg, inputs)
    nc = build(cfg, maps[0])
    res = run_bass_kernel_spmd(nc, maps, core_ids=list(range(NCORES)))
    out = np.concatenate([np.asarray(res.results[r]["out"], np.float32) for r in range(NCORES)], 0)
    return out.reshape(cfg.NB, cfg.L, cfg.D)
```
